# Optimizing a Trainium2 kernel written in Bass

```python
import math
import jax, jax.numpy as jnp
from jax import lax
import numpy as np

D_MODEL = 4096
BATCH = 2
SEQ = 4096
DEPTH = 2

CONV_CH = D_MODEL // 2
CONV_WIDTH = 31
HEAD_DIM = 128
HEADS_PER_GROUP = 8
ATTN_GROUPS = ((128, 1), (512, 4), (2048, 16))
N_GROUPS = len(ATTN_GROUPS)
N_ATT_HEADS = N_GROUPS * HEADS_PER_GROUP
ATT_W = N_ATT_HEADS * HEAD_DIM
ATT_OUT_W = HEADS_PER_GROUP * HEAD_DIM
BLOCK = 128
NUM_BUCKETS = 32
MAX_DISTANCE = 2048
IN_W = 2 * CONV_CH + 3 * ATT_W + 2 * D_MODEL
D_FF = 11008
N_EXPERTS = 8
TOP_K = 2
D_FF_EXPERT = 3584
N_DENSE = (DEPTH + 1) // 2
N_MOE = DEPTH // 2

kernel_name = 'hybrid_conv_dilated_attn_moe_block'


def rms_norm(x, g, eps=1e-6):
    xf = x.astype(jnp.float32)
    y = xf * lax.rsqrt(jnp.mean(xf * xf, axis=-1, keepdims=True) + eps)
    return (y * g.astype(jnp.float32)).astype(x.dtype)


def layer_norm(x, g, b, eps=1e-5):
    xf = x.astype(jnp.float32)
    mu = jnp.mean(xf, axis=-1, keepdims=True)
    var = jnp.mean(jnp.square(xf - mu), axis=-1, keepdims=True)
    y = (xf - mu) * lax.rsqrt(var + eps)
    return (y * g.astype(jnp.float32) + b.astype(jnp.float32)).astype(x.dtype)


def modulate(h, shift, scale):
    return h * (1 + scale[:, None, :]) + shift[:, None, :]


def t5_bucket(dist):
    max_exact = NUM_BUCKETS // 2
    n = jnp.maximum(dist, 1).astype(jnp.float32)
    large = max_exact + (jnp.log(n / max_exact) / math.log(MAX_DISTANCE / max_exact)
                         * (NUM_BUCKETS - max_exact)).astype(jnp.int32)
    large = jnp.minimum(large, NUM_BUCKETS - 1)
    return jnp.where(dist < max_exact, dist, large)


def conv_module(glu_in, conv_w, conv_b, ln_g, ln_b, w_conv_out):
    a, b = jnp.split(glu_in, 2, axis=-1)
    u = a * jax.nn.sigmoid(b)
    u = lax.conv_general_dilated(u, conv_w[:, None, :].astype(u.dtype), window_strides=(1,),
                                 padding=[(CONV_WIDTH - 1, 0)],
                                 dimension_numbers=('NWC', 'WIO', 'NWC'),
                                 feature_group_count=CONV_CH) + conv_b
    u = jax.nn.silu(layer_norm(u, ln_g, ln_b))
    return u @ w_conv_out


def dilated_attention(q, k, v, rel_table, window, dilation):
    B, S, H, hd = q.shape
    L = S // dilation
    span = window // dilation
    blk = min(BLOCK, L)
    nb = -(-L // blk)
    Lp = nb * blk

    def to_sub(t):
        t = t.reshape(B, L, dilation, H, hd).transpose(0, 2, 1, 3, 4)
        t = jnp.pad(t, ((0, 0), (0, 0), (0, Lp - L), (0, 0), (0, 0)))
        return t.reshape(B, dilation, nb, blk, H, hd)

    def with_prev(t):
        prev = jnp.pad(t, ((0, 0), (0, 0), (1, 0), (0, 0), (0, 0), (0, 0)))[:, :, :-1]
        return jnp.concatenate([prev, t], axis=3)

    qs = to_sub(q)
    kw = with_prev(to_sub(k))
    vw = with_prev(to_sub(v))
    scores = jnp.einsum('brnqhd,brnkhd->brnhqk', qs, kw,
                        preferred_element_type=jnp.float32) * (HEAD_DIM ** -0.5)
    qi = jnp.arange(blk)[:, None] + blk
    kj = jnp.arange(2 * blk)[None, :]
    delta = qi - kj
    band = (delta >= 0) & (delta <= span)
    valid = band[None] & ((jnp.arange(nb)[:, None, None] > 0) | (kj >= blk)[None])
    bias = rel_table[t5_bucket(jnp.maximum(delta, 0) * dilation)]
    scores = scores + bias.astype(jnp.float32).transpose(2, 0, 1)
    scores = jnp.where(valid[:, None], scores, -jnp.inf)
    lse = jax.nn.logsumexp(scores, axis=-1)
    p = jnp.exp(scores - lse[..., None])
    out = jnp.einsum('brnhqk,brnkhd->brnqhd', p, vw.astype(jnp.float32))
    out = out.reshape(B, dilation, Lp, H, hd)[:, :, :L].transpose(0, 2, 1, 3, 4).reshape(B, S, H, hd)
    lse = lse.transpose(0, 1, 2, 4, 3).reshape(B, dilation, Lp, H)[:, :, :L]
    lse = lse.transpose(0, 2, 1, 3).reshape(B, S, H)
    return out, lse


def hybrid_mixer(h, w_in, conv_w, conv_b, ln_g, ln_b, w_conv_out, q_gain, k_gain, rel_bias,
                 w_attn_out, w_out):
    B, S, _ = h.shape
    p = h @ w_in
    o1 = 2 * CONV_CH
    cuts = [o1, o1 + ATT_W, o1 + 2 * ATT_W, o1 + 3 * ATT_W, o1 + 3 * ATT_W + D_MODEL]
    glu_in, q, k, v, gc, ga = jnp.split(p, cuts, axis=-1)
    conv_out = conv_module(glu_in, conv_w, conv_b, ln_g, ln_b, w_conv_out)
    shp = (B, S, N_GROUPS, HEADS_PER_GROUP, HEAD_DIM)
    q = rms_norm(q.reshape(shp), q_gain)
    k = rms_norm(k.reshape(shp), k_gain)
    v = v.reshape(shp)
    outs, lses = [], []
    for g, (window, dil) in enumerate(ATTN_GROUPS):
        o, lse = dilated_attention(q[:, :, g], k[:, :, g], v[:, :, g],
                                   rel_bias[:, g * HEADS_PER_GROUP:(g + 1) * HEADS_PER_GROUP],
                                   window, dil)
        outs.append(o)
        lses.append(lse)
    wgt = jax.nn.softmax(jnp.stack(lses), axis=0)
    o = jnp.sum(wgt[..., None] * jnp.stack(outs), axis=0).reshape(B, S, ATT_OUT_W).astype(h.dtype)
    attn_out = o @ w_attn_out
    merged = jax.nn.sigmoid(gc) * conv_out + jax.nn.sigmoid(ga) * attn_out
    return merged @ w_out


def swiglu(h, w1, w3, w2):
    return (jax.nn.silu(h @ w1) * (h @ w3)) @ w2


def moe_ffn(h, w_router, w1, w3, w2):
    logits = jnp.einsum('bsd,de->bse', h, w_router, preferred_element_type=jnp.float32)
    top_v, top_i = lax.top_k(logits, TOP_K)
    top_w = jax.nn.softmax(top_v, axis=-1)
    comb = jnp.sum(jax.nn.one_hot(top_i, N_EXPERTS, dtype=jnp.float32) * top_w[..., None], axis=-2)
    y = jnp.zeros(h.shape, jnp.float32)
    for e in range(N_EXPERTS):
        y = y + comb[..., e:e + 1] * swiglu(h, w1[e], w3[e], w2[e])
    return y.astype(h.dtype)


def setup_inputs(seed: int = 0) -> dict:
    key = jax.random.key(seed)
    ks = jax.random.split(key, 26)
    nrm = jax.random.normal
    D = D_MODEL
    f32 = jnp.float32
    return {
        'x': nrm(ks[0], (BATCH, SEQ, D), f32),
        'c': nrm(ks[1], (BATCH, D), f32),
        'w_ada': nrm(ks[2], (DEPTH, D, 6 * D), f32) * (0.5 * D ** -0.5),
        'b_ada': nrm(ks[3], (DEPTH, 6 * D), f32) * 0.02,
        'g_mix': 1.0 + 0.02 * nrm(ks[4], (DEPTH, D), f32),
        'g_ffn': 1.0 + 0.02 * nrm(ks[5], (DEPTH, D), f32),
        'w_in': nrm(ks[6], (DEPTH, D, IN_W), f32) * D ** -0.5,
        'conv_w': nrm(ks[7], (DEPTH, CONV_WIDTH, CONV_CH), f32) * CONV_WIDTH ** -0.5,
        'conv_b': nrm(ks[8], (DEPTH, CONV_CH), f32) * 0.02,
        'conv_ln_g': 1.0 + 0.02 * nrm(ks[9], (DEPTH, CONV_CH), f32),
        'conv_ln_b': nrm(ks[10], (DEPTH, CONV_CH), f32) * 0.02,
        'w_conv_out': nrm(ks[11], (DEPTH, CONV_CH, D), f32) * CONV_CH ** -0.5,
        'q_gain': 1.0 + 0.02 * nrm(ks[12], (DEPTH, HEAD_DIM), f32),
        'k_gain': 1.0 + 0.02 * nrm(ks[13], (DEPTH, HEAD_DIM), f32),
        'rel_bias': nrm(ks[14], (NUM_BUCKETS, N_ATT_HEADS), f32) * 0.2,
        'w_attn_out': nrm(ks[15], (DEPTH, ATT_OUT_W, D), f32) * ATT_OUT_W ** -0.5,
        'w_out': nrm(ks[16], (DEPTH, D, D), f32) * D ** -0.5,
        'ffn_w1': nrm(ks[17], (N_DENSE, D, D_FF), f32) * D ** -0.5,
        'ffn_w3': nrm(ks[18], (N_DENSE, D, D_FF), f32) * D ** -0.5,
        'ffn_w2': nrm(ks[19], (N_DENSE, D_FF, D), f32) * D_FF ** -0.5,
        'moe_router': nrm(ks[20], (N_MOE, D, N_EXPERTS), f32) * D ** -0.5,
        'moe_w1': nrm(ks[21], (N_MOE, N_EXPERTS, D, D_FF_EXPERT), f32) * D ** -0.5,
        'moe_w3': nrm(ks[22], (N_MOE, N_EXPERTS, D, D_FF_EXPERT), f32) * D ** -0.5,
        'moe_w2': nrm(ks[23], (N_MOE, N_EXPERTS, D_FF_EXPERT, D), f32) * D_FF_EXPERT ** -0.5,
    }


def reference(x, c, w_ada, b_ada, g_mix, g_ffn, w_in, conv_w, conv_b, conv_ln_g, conv_ln_b,
              w_conv_out, q_gain, k_gain, rel_bias, w_attn_out, w_out, ffn_w1, ffn_w3, ffn_w2,
              moe_router, moe_w1, moe_w3, moe_w2):
    c_act = jax.nn.silu(c)
    for l in range(DEPTH):
        mod = c_act @ w_ada[l] + b_ada[l]
        sh1, sc1, gt1, sh2, sc2, gt2 = jnp.split(mod, 6, axis=-1)
        h = modulate(rms_norm(x, g_mix[l]), sh1, sc1)
        y = hybrid_mixer(h, w_in[l], conv_w[l], conv_b[l], conv_ln_g[l], conv_ln_b[l],
                         w_conv_out[l], q_gain[l], k_gain[l], rel_bias, w_attn_out[l], w_out[l])
        x = x + gt1[:, None, :] * y
        h = modulate(rms_norm(x, g_ffn[l]), sh2, sc2)
        if l % 2 == 0:
            j = l // 2
            y = swiglu(h, ffn_w1[j], ffn_w3[j], ffn_w2[j])
        else:
            j = l // 2
            y = moe_ffn(h, moe_router[j], moe_w1[j], moe_w3[j], moe_w2[j])
        x = x + gt2[:, None, :] * y
    return x
```

```python
import contextlib
import os
import numpy as np
import concourse.bass as bass
import concourse.mybir as mybir
from concourse.bass_utils import run_bass_kernel_spmd

F32 = mybir.dt.float32
BF16 = mybir.dt.bfloat16
AF = mybir.ActivationFunctionType
ALU = mybir.AluOpType
AX = mybir.AxisListType

NCORE = 8
D = 4096
KC = 32
T = 1024
NH = 24
CCH = 16
CW = 31
HALO = 32
DFF_C = 86
EXP = 8
DFE_C = 28
FFG = [(0, 32), (32, 64), (64, 86)]
DIL = [1, 4, 16]
UNIT_COLS = KC * 128


class Eng:
    def __init__(self, name, h, sem, step, is_dma, issuer=None):
        self.name, self.h, self.sem, self.step, self.is_dma = name, h, sem, step, is_dma
        self.cnt = 0
        self.issuer = issuer or self
        self.seen = {}


class Tk:
    __slots__ = ("name", "w", "r")

    def __init__(self, name=""):
        self.name, self.w, self.r = name, {}, {}


class MK:
    def __init__(self):
        self.nc = bass.Bass("TRN2", target_bir_lowering=False)
        self.es = contextlib.ExitStack()
        nc = self.nc
        self.n_instr = 0
        self.nsem = 0
        self.pe = Eng("pe", nc.tensor, self._sem("s_pe"), 1, False)
        self.act = Eng("act", nc.scalar, self._sem("s_act"), 1, False)
        self.dve = Eng("dve", nc.vector, self._sem("s_dve"), 1, False)
        self.pool = Eng("pool", nc.gpsimd, self._sem("s_pool"), 1, False)
        self.sp = Eng("sp", nc.sync, self._sem("s_sp"), 1, False)
        self.chans = []

    def _sem(self, n):
        self.nsem += 1
        return self.es.enter_context(self.nc.semaphore(n))

    def chan(self, name, issuer=None):
        issuer = issuer or self.sp
        c = Eng(name, issuer.h, self._sem("c_" + name), 16, True, issuer=issuer)
        self.chans.append(c)
        return c

    def cc_chan(self, name):
        c = Eng(name, self.pool.h, self._sem("cc_" + name), 1, True, issuer=self.pool)
        self.chans.append(c)
        return c

    def sbuf(self, name, shape, dt):
        return self.es.enter_context(self.nc.sbuf_tensor(name, list(shape), dt))

    def psum(self, name, shape, dt=F32):
        return self.es.enter_context(self.nc.psum_tensor(name, list(shape), dt))

    def _wait(self, eng, prod, cnt):
        iss = eng.issuer
        if iss.seen.get(prod, 0) >= cnt:
            return
        iss.h.wait_ge(prod.sem, cnt)
        self.n_instr += 1
        iss.seen[prod] = cnt

    def op(self, eng, fn, reads=(), writes=(), signal=True):
        deps = {}

        def need(p, c):
            if c > deps.get(p, 0):
                deps[p] = c

        for t in reads:
            for p, c in t.w.items():
                if p is eng and eng is self.pe:
                    continue
                need(p, c)
        for t in writes:
            for p, c in t.w.items():
                if p is eng and not eng.is_dma:
                    continue
                need(p, c)
            for p, c in t.r.items():
                if p is eng and not eng.is_dma:
                    continue
                need(p, c)
        if eng.is_dma and eng.cnt:
            need(eng, eng.cnt)
        for p, c in deps.items():
            self._wait(eng, p, c)
        ins = fn()
        self.n_instr += 1
        if signal:
            ins.then_inc(eng.sem, eng.step)
            eng.cnt += eng.step
            mark = eng.cnt
        else:
            mark = eng.cnt + eng.step
        for t in writes:
            t.w = {eng: mark}
            t.r = {}
        for t in reads:
            if t.r.get(eng, 0) < mark:
                t.r[eng] = mark
        return ins

    def add_writer(self, t, eng):
        pass

    def close(self):
        self.es.close()


def _tiles(W):
    K, N = W.shape
    return np.ascontiguousarray(W.reshape(K // 128, 128, N // 128, 128).transpose(2, 1, 0, 3))


def unit_plan(cfg):
    segs = []
    for l in cfg["layers"]:
        segs.append(("ada%d" % l, 192))
        segs.append(("glu%d" % l, 32))
        segs.append(("qkv%d" % l, 72))
        if cfg["stop"] in ("qkv", "xch", "conv", "attn") and l == cfg["layers"][-1]:
            break
        segs.append(("mrg%d" % l, 128))
        segs.append(("out%d" % l, 32))
        if cfg["stop"] == "mix" and l == cfg["layers"][-1]:
            break
        if l == 0:
            segs.append(("ffn", 2 * DFF_C + 96))
        else:
            segs.append(("moe", EXP * (2 * DFE_C + 32)))
    return segs


def build_units(inp, cfg):
    segs = unit_plan(cfg)
    total = sum(n for _, n in segs)
    total_p = -(-total // 8) * 8
    seq = np.zeros((total_p, 128, KC, 128), np.float32)
    base = 0
    for tag, n in segs:
        if tag.startswith("ada"):
            l = int(tag[3:])
            seq[base:base + 192] = _tiles(inp["w_ada"][l])
        elif tag.startswith("glu"):
            l = int(tag[3:])
            wt = _tiles(inp["w_in"][l][:, 0:4096])
            seq[base:base + 32:2] = wt[0:16]
            seq[base + 1:base + 32:2] = wt[16:32]
        elif tag.startswith("qkv"):
            l = int(tag[3:])
            wt = _tiles(inp["w_in"][l][:, 4096:13312])
            seq[base:base + 72:3] = wt[0:24]
            seq[base + 1:base + 72:3] = wt[24:48]
            seq[base + 2:base + 72:3] = wt[48:72]
        elif tag.startswith("mrg"):
            l = int(tag[3:])
            wt = _tiles(inp["w_in"][l][:, 13312:21504])
            seq[base:base + 128:4] = wt[0:32]
            seq[base + 1:base + 128:4] = wt[32:64]
            seq[base + 2:base + 128:4, :, 0:16] = _tiles(inp["w_conv_out"][l])
            seq[base + 3:base + 128:4, :, 0:8] = _tiles(inp["w_attn_out"][l])
        elif tag.startswith("out"):
            l = int(tag[3:])
            seq[base:base + 32] = _tiles(inp["w_out"][l])
        elif tag == "ffn":
            w1 = _tiles(inp["ffn_w1"][0]); w3 = _tiles(inp["ffn_w3"][0]); w2 = _tiles(inp["ffn_w2"][0])
            b = base
            for (f0, f1) in FFG:
                nf = f1 - f0
                seq[b:b + 2 * nf:2] = w1[f0:f1]
                seq[b + 1:b + 2 * nf:2] = w3[f0:f1]
                b += 2 * nf
                seq[b:b + 32, :, 0:nf] = w2[:, :, f0:f1]
                b += 32
        elif tag == "moe":
            b = base
            for e in range(EXP):
                w1 = _tiles(inp["moe_w1"][0, e]); w3 = _tiles(inp["moe_w3"][0, e]); w2 = _tiles(inp["moe_w2"][0, e])
                seq[b:b + 2 * DFE_C:2] = w1
                seq[b + 1:b + 2 * DFE_C:2] = w3
                b += 2 * DFE_C
                seq[b:b + 32, :, 0:DFE_C] = w2
                b += 32
        base += n
    ncol = total_p // 8
    seq = seq.reshape(ncol, 8, 128, UNIT_COLS)
    return [np.ascontiguousarray(seq[:, r]) for r in range(NCORE)], ncol


def t5_bucket_np(dist):
    dist = np.asarray(dist)
    n = np.maximum(dist, 1).astype(np.float32)
    large = 16 + (np.log(n / np.float32(16)) / np.float32(np.log(2048 / 16)) * np.float32(16)).astype(np.int32)
    large = np.minimum(large, 31)
    return np.where(dist < 16, dist, large)


def bias_layout(rel_bias):
    k = np.arange(128)[:, None]
    q = np.arange(128)[None, :]
    bd = np.zeros((128, NH, 128), np.float32)
    bp = np.zeros((128, NH, 128), np.float32)
    for g in range(3):
        idxD = t5_bucket_np(np.maximum(q - k, 0) * DIL[g])
        idxP = t5_bucket_np(np.clip(q + 128 - k, 0, 128) * DIL[g])
        for hh in range(8):
            h = g * 8 + hh
            bd[:, h, :] = rel_bias[idxD, h]
            bp[:, h, :] = rel_bias[idxP, h]
    return bd, bp


def vec_pc(v):
    return np.ascontiguousarray(v.reshape(-1, 128).T)


XSPEC = {"k3a": (512, 1024), "k3b": (512, 1024), "v3a": (512, 1024), "v3b": (512, 1024),
         "k2": (1024, 512), "v2": (512, 1024), "x1": (2560, 128)}


def build(cfg, ncol):
    m = MK()
    nc = m.nc
    layers = cfg["layers"]
    stop = cfg["stop"]
    dbg = cfg.get("dbg", False)
    NL = 2
    segs = unit_plan(cfg)
    total_units = sum(n for _, n in segs)
    do_moe = (1 in layers and stop == "all")

    def units_through(tag):
        n = 0
        for t_, k_ in segs:
            n += k_
            if t_ == tag:
                return n
        return n

    def din(name, shape, dt=F32):
        return nc.dram_tensor(name, list(shape), dt, kind="ExternalInput").ap()

    def dscr(name, shape, dt):
        return nc.dram_tensor(name, list(shape), dt, kind="Internal").ap()

    def dout(name, shape, dt=F32):
        return nc.dram_tensor(name, list(shape), dt, kind="ExternalOutput").ap()

    xT_in = din("xT", [D, T])
    wsh = din("wsh", [ncol, 128, UNIT_COLS])
    cvec = din("cvec", [128, KC])
    bada = din("bada", [NL, 128, 192])
    gmix = din("gmix", [NL, 128, KC])
    gffn = din("gffn", [NL, 128, KC])
    convw = din("convw", [NL, 128, CCH, CW])
    convp = din("convp", [NL, 128, 3, CCH])
    qkg = din("qkg", [NL, 128, 2])
    biasD = din("biasD", [128, NH, 128])
    biasP = din("biasP", [128, 16, 128])
    biasG = din("biasG", [128, 8, 3, 128])
    cst = din("cst", [128, 7, 128])
    sel_in = din("sel", [8, EXP * 128])
    wr_in = din("wr", [128, KC, EXP])
    outT = dout("outT", [D, T])

    ACH = 24
    arena_t = [dscr("arena%d" % i, [min(ACH, ncol - ACH * i), 8 * 128, UNIT_COLS], BF16) for i in range(-(-ncol // ACH))]

    def arena_at(c):
        return arena_t[c // ACH][c % ACH]
    NSRC = 3
    srcb = [dscr("srcb%d" % i, [128, UNIT_COLS], BF16) for i in range(NSRC)]
    qs = dscr("qs", [NH, 128, T], BF16)
    ks = dscr("ks", [NH, 128, T], BF16)
    vs = dscr("vs", [T, NH * 128], BF16)
    mscr = dscr("mscr", [KC, 128, T], BF16)
    xsrc, xdst = {}, {}
    for l in layers:
        for nme, (R, C) in XSPEC.items():
            xsrc[(l, nme)] = dscr("xs_%s_%d" % (nme, l), [R, C], BF16)
            xdst[(l, nme)] = dscr("xd_%s_%d" % (nme, l), [10 * R, C], BF16)

    Rh = m.sbuf("Rh", [128, KC, T], BF16)
    RB_COLS = 82944 // 2
    Rb = m.sbuf("Rb", [128, RB_COLS], BF16)
    UW = HALO + T
    U_OFF, CU_OFF, O_OFF = 0, CCH * UW, CCH * UW + CCH * T
    uh = Rb[:, U_OFF:CU_OFF].rearrange("p (c t) -> p c t", c=CCH)
    cuT = Rb[:, CU_OFF:O_OFF].rearrange("p (c t) -> p c t", c=CCH)
    oT = Rb[:, O_OFF:O_OFF + 8 * T].rearrange("p (c t) -> p c t", c=8)
    h1 = Rb[:, 0:32 * T].rearrange("p (c t) -> p c t", c=32)
    NWB = 3
    wb = [m.sbuf("wb%d" % i, [128, KC, 128], BF16) for i in range(NWB)]
    NXC = 3
    xc = [m.sbuf("xc%d" % i, [128, T], F32) for i in range(NXC)]
    NTMP = 4
    tmp = [m.sbuf("tmp%d" % i, [128, 512], F32) for i in range(NTMP)]
    modv = m.sbuf("modv", [128, 192], F32)
    badat = m.sbuf("badat", [128, 192], F32)
    avec = m.sbuf("avec", [128, 2, KC], F32)
    gvec = m.sbuf("gvec", [128, 2, KC], F32)
    cact = m.sbuf("cact", [128, KC], BF16)
    cf32 = m.sbuf("cf32", [128, KC], F32)
    cwt = m.sbuf("cwt", [128, CCH, CW], F32)
    cpt = m.sbuf("cpt", [128, 3, CCH], F32)
    qkgt = m.sbuf("qkgt", [128, 4], F32)
    cstt = m.sbuf("cstt", [128, 7, 128], F32)
    identb = m.sbuf("identb", [128, 128], BF16)
    onesb = m.sbuf("onesb", [128, 128], BF16)
    onesf = m.sbuf("onesf", [128, 128], F32)
    zrow = m.sbuf("zrow", [128, 512], BF16)
    small = m.sbuf("small", [128, 16], F32)
    M0 = 32 * T
    selt = Rb[0:8, M0:M0 + 2048].bitcast(F32); M0 += 2048
    combT = Rb[0:8, M0:M0 + 2048].bitcast(F32); M0 += 2048
    LT = Rb[0:8, M0:M0 + 2048].bitcast(F32); M0 += 2048
    wrt = Rb[:, M0:M0 + 512].bitcast(F32).rearrange("p (k e) -> p k e", e=EXP); M0 += 512
    Ltm = Rb[:, M0:M0 + 128].bitcast(F32); M0 += 128
    ctm = Rb[:, M0:M0 + 128].bitcast(F32); M0 += 128
    assert M0 <= RB_COLS
    A0 = U_OFF
    MDP = Rb[:, A0:A0 + 16 * 256].rearrange("p (h q) -> p h q", h=16); A0 += 16 * 256
    MG3 = Rb[:, A0:A0 + 8 * 384].rearrange("p (h w q) -> p h w q", h=8, w=3); A0 += 8 * 384
    Qh = Rb[:, A0:A0 + T]; A0 += T
    K0 = Rb[:, A0:A0 + T]; A0 += T
    KA = Rb[:, A0:A0 + 2 * T]; A0 += 2 * T
    V0 = Rb[:, A0:A0 + T]; A0 += T
    VA = Rb[:, A0:A0 + 2 * T]; A0 += 2 * T
    NPT = 4
    PT = [Rb[:, A0 + i * 256:A0 + (i + 1) * 256] for i in range(NPT)]; A0 += NPT * 256
    assert A0 <= CU_OFF, (A0, CU_OFF)
    mgt = [Rb[:, U_OFF + i * T:U_OFF + (i + 1) * T] for i in range(2)]

    ps = [m.psum("ps%d" % i, [128, 512]) for i in range(8)]
    ps_k = [Tk("ps%d" % i) for i in range(8)]

    Rh_k = Tk("Rh")
    uh_k, cu_k, o_k, h1_k = Tk("uh"), Tk("cuT"), Tk("oT"), Tk("h1")
    wb_k = [Tk("wb%d" % i) for i in range(NWB)]
    wb_c = [m.chan("wb%d" % i) for i in range(NWB)]
    xc_k = [Tk("xc%d" % i) for i in range(NXC)]
    xc_c = [m.chan("xc%d" % i) for i in range(NXC)]
    tmp_k = [Tk("tmp%d" % i) for i in range(NTMP)]
    x_k = [Tk("x%d" % j) for j in range(KC)]
    mod_k, av_k, misc_k = Tk("modv"), Tk("avec"), Tk("misc")
    misc_c = m.chan("misc")
    srcb_k = [Tk("srcb%d" % i) for i in range(NSRC)]
    srcb_c = [m.chan("srcb%d" % i, issuer=m.pool) for i in range(NSRC)]
    NCC = 3
    ccc = [m.cc_chan("cc%d" % i) for i in range(NCC)]
    col_k = [Tk("col%d" % c) for c in range(ncol)]
    cc_hist = []
    core = nc.sync.partition_id()
    dynbase = {}
    SP, ACT, DVE, PE, POOL = m.sp, m.act, m.dve, m.pe, m.pool

    rb_all = [h1_k, uh_k, cu_k, o_k]
    sm_k = Tk("small")

    def retire(*tks):
        t = Tk("ret")
        for k_ in tks:
            for p_, c_ in list(k_.w.items()) + list(k_.r.items()):
                if t.w.get(p_, 0) < c_:
                    t.w[p_] = c_
        return t

    def dma(ch, out, in_, reads=(), writes=()):
        return m.op(ch, lambda: ch.issuer.h.dma_start(out=out, in_=in_), reads=reads, writes=writes)

    dmy = m.sbuf("dmy", [128, 8], F32)
    ps_set = set(id(t) for t in ps_k)

    def V(fn, reads, writes):
        if any(id(t) in ps_set for t in reads):
            m.op(DVE, fn, reads=reads, writes=writes, signal=False)
            return m.op(DVE, lambda: nc.vector.tensor_copy(out=dmy[0:1, 0:1], in_=dmy[0:1, 1:2]))
        return m.op(DVE, fn, reads=reads, writes=writes)

    def A(fn, reads, writes):
        if any(id(t) in ps_set for t in reads):
            m.op(ACT, fn, reads=reads, writes=writes, signal=False)
            return m.op(ACT, lambda: nc.scalar.copy(out=dmy[0:1, 4:5], in_=dmy[0:1, 5:6]))
        return m.op(ACT, fn, reads=reads, writes=writes)

    dma(misc_c, cstt[:], cst, writes=[misc_k])
    V(lambda: nc.vector.tensor_copy(out=identb[:], in_=cstt[:, 2, :]), [misc_k], [misc_k])
    V(lambda: nc.vector.memset(onesb[:], 1.0), [], [misc_k])
    V(lambda: nc.vector.memset(onesf[:], 1.0), [], [misc_k])
    V(lambda: nc.vector.memset(zrow[:], 0.0), [], [misc_k])
    V(lambda: nc.vector.memset(dmy[:], 0.0), [], [misc_k])
    dma(misc_c, cf32[:], cvec, writes=[misc_k])
    A(lambda: nc.scalar.activation(out=cact[:], in_=cf32[:], func=AF.Silu), [misc_k], [misc_k])

    cc_state = {"n": 0}

    def collective(src_ap, dst_ap, reads, writes):
        i = cc_state["n"]
        cc_state["n"] += 1
        ch = ccc[i % NCC]
        if len(cc_hist) >= 2:
            p, c = cc_hist[-2]
            m._wait(ch, p, c)
        m.op(ch, lambda: nc.gpsimd.collective_compute("AllGather", ALU.bypass, replica_groups=[list(range(NCORE))],
                                                      ins=[src_ap], outs=[dst_ap]), reads=reads, writes=writes)
        cc_hist.append((ch, ch.cnt))

    gat = {"next": 0}

    def gather_upto(c_end):
        while gat["next"] < min(c_end, ncol):
            c = gat["next"]
            gat["next"] += 1
            b = c % NSRC
            dma(srcb_c[b], srcb[b], wsh[c], reads=[], writes=[srcb_k[b]])
            collective(srcb[b].opt(), arena_at(c).opt(), [srcb_k[b]], [col_k[c]])

    wq = {"req": 0, "pending": []}

    def w_request():
        u = wq["req"]
        wq["req"] += 1
        c, q = u // 8, u % 8
        b = u % NWB
        dma(wb_c[b], wb[b][:].rearrange("p k n -> p (k n)"), arena_at(c)[q * 128:(q + 1) * 128, :],
            reads=[col_k[c]], writes=[wb_k[b]])
        wq["pending"].append(b)

    def w_prefetch():
        while len(wq["pending"]) < NWB - 1 and wq["req"] < total_units:
            w_request()

    def w_next():
        w_prefetch()
        if not wq["pending"]:
            w_request()
        return wq["pending"].pop(0)

    def mm_unit(b, nkc, rhs3, rhs_k, banks, first=True, last=True):
        for hf, bank in enumerate(banks):
            for kc in range(nkc):
                m.op(PE, lambda: nc.tensor.matmul(ps[bank][:], lhsT=wb[b][:, kc, :], rhs=rhs3[:, kc, hf * 512:(hf + 1) * 512],
                                                  start=(first and kc == 0), stop=(last and kc == nkc - 1)),
                     reads=[wb_k[b], rhs_k], writes=[ps_k[bank]], signal=(kc == nkc - 1))
        w_prefetch()

    cnt = {"tmp": 0, "xc": 0}

    def tmp_get():
        i = cnt["tmp"] % NTMP
        cnt["tmp"] += 1
        return i

    def xc_get(avoid=()):
        while True:
            i = cnt["xc"] % NXC
            cnt["xc"] += 1
            if i not in avoid:
                return i

    def HS(hf):
        return slice(hf * 512, (hf + 1) * 512)

    def x_rmw(j, banks, gt_off):
        xi = xc_get(avoid=rmw_avoid)
        src = rmw_src(j)
        dma(xc_c[xi], xc[xi][:], src, reads=[x_k[j]], writes=[xc_k[xi]])
        for hf, bank in enumerate(banks):
            V(lambda: nc.vector.scalar_tensor_tensor(out=xc[xi][:, HS(hf)], in0=ps[bank][:], scalar=modv[:, gt_off + j:gt_off + j + 1],
                                                     in1=xc[xi][:, HS(hf)], op0=ALU.mult, op1=ALU.add),
              [ps_k[bank], xc_k[xi], mod_k], [xc_k[xi]])
        dma(xc_c[xi], outT[j * 128:(j + 1) * 128, :], xc[xi][:], reads=[xc_k[xi]], writes=[x_k[j]])

    rmw_avoid = ()
    rmw_src = None

    for l in layers:
        if l == 0:
            last_tag = "ffn" if stop in ("ffn", "all") else ("out0" if stop == "mix" else "qkv0")
        else:
            last_tag = "out1" if stop in ("mix", "ffn", "all") else "qkv1"
        gather_upto(-(-units_through(last_tag) // 8))
        rb_dead = retire(*rb_all)

        def x_in(j, l=l):
            return xT_in[j * 128:(j + 1) * 128, :] if l == 0 else outT[j * 128:(j + 1) * 128, :]

        dma(misc_c, badat[:], bada[l], writes=[misc_k])
        dma(misc_c, gvec[:, 0, :], gmix[l], writes=[misc_k])
        dma(misc_c, gvec[:, 1, :], gffn[l], writes=[misc_k])
        dma(misc_c, cwt[:], convw[l], writes=[misc_k])
        dma(misc_c, cpt[:], convp[l], writes=[misc_k])
        dma(misc_c, qkgt[:, 0:2], qkg[l], writes=[misc_k])
        V(lambda: nc.vector.tensor_scalar(out=qkgt[:, 2:3], in0=qkgt[:, 1:2], scalar1=float(np.sqrt(128.0)), scalar2=None, op0=ALU.mult),
          [misc_k], [misc_k])

        MB = 4
        for jc in range(192):
            b = w_next()
            for kc in range(KC):
                m.op(PE, lambda: nc.tensor.matmul(ps[MB][:, jc:jc + 1], lhsT=wb[b][:, kc, :], rhs=cact[:, kc:kc + 1],
                                                  start=(kc == 0), stop=(kc == KC - 1), skip_group_check=True),
                     reads=[wb_k[b], misc_k], writes=[ps_k[MB]], signal=(kc == KC - 1))
            w_prefetch()
        V(lambda: nc.vector.tensor_tensor(out=modv[:], in0=ps[MB][:, 0:192], in1=badat[:], op=ALU.add), [ps_k[MB], misc_k], [mod_k])
        for i, off in enumerate((32, 128)):
            V(lambda: nc.vector.scalar_tensor_tensor(out=avec[:, i, :], in0=modv[:, off:off + 32], scalar=1.0, in1=gvec[:, i, :],
                                                     op0=ALU.add, op1=ALU.mult), [mod_k, misc_k], [av_k])
        SH1, GT1, SH2, GT2 = 0, 64, 96, 160
        if dbg:
            dbg_mod = dout("dbg_mod%d" % l, [128, 192])
            dma(misc_c, dbg_mod, modv[:], reads=[mod_k])

        def norm_phase(which, src_fn, router=False):
            sh_off = SH1 if which == 0 else SH2
            SB = (5, 6)
            for kc in range(KC):
                xi = xc_get()
                dma(xc_c[xi], xc[xi][:], src_fn(kc), reads=[x_k[kc]], writes=[xc_k[xi]])
                for hf, bank in enumerate(SB):
                    ti = tmp_get()
                    A(lambda: nc.scalar.activation(out=tmp[ti][:], in_=xc[xi][:, HS(hf)], func=AF.Square), [xc_k[xi]], [tmp_k[ti]])
                    m.op(PE, lambda: nc.tensor.matmul(ps[bank][:], lhsT=onesf[:], rhs=tmp[ti][:], start=(kc == 0), stop=(kc == KC - 1)),
                         reads=[tmp_k[ti], misc_k], writes=[ps_k[bank]])
            ri = xc_get()
            rstd = xc[ri]
            for hf, bank in enumerate(SB):
                A(lambda: nc.scalar.activation(out=rstd[:, HS(hf)], in_=ps[bank][:], func=AF.Sqrt, bias=1e-6, scale=1.0 / D),
                  [ps_k[bank]], [xc_k[ri]])
                V(lambda: nc.vector.reciprocal(out=rstd[:, HS(hf)], in_=rstd[:, HS(hf)]), [xc_k[ri]], [xc_k[ri]])
            for kc in range(KC):
                xi = xc_get(avoid=(ri,))
                dma(xc_c[xi], xc[xi][:], src_fn(kc), reads=[x_k[kc]], writes=[xc_k[xi]])
                for hf in range(2):
                    ti = tmp_get()
                    V(lambda: nc.vector.scalar_tensor_tensor(out=tmp[ti][:], in0=xc[xi][:, HS(hf)], scalar=avec[:, which, kc:kc + 1],
                                                             in1=rstd[:, HS(hf)], op0=ALU.mult, op1=ALU.mult),
                      [xc_k[xi], xc_k[ri], av_k], [tmp_k[ti]])
                    A(lambda: nc.scalar.activation(out=Rh[:, kc, HS(hf)], in_=tmp[ti][:], func=AF.Identity,
                                                   bias=modv[:, sh_off + kc:sh_off + kc + 1], scale=1.0), [tmp_k[ti], mod_k], [Rh_k])
                    if router:
                        t2 = tmp_get()
                        A(lambda: nc.scalar.activation(out=tmp[t2][:], in_=tmp[ti][:], func=AF.Identity,
                                                       bias=modv[:, sh_off + kc:sh_off + kc + 1], scale=1.0), [tmp_k[ti], mod_k], [tmp_k[t2]])
                        m.op(PE, lambda: nc.tensor.matmul(ps[3 + hf][0:8, :], lhsT=wrt[:, kc, :], rhs=tmp[t2][:],
                                                          start=(kc == 0), stop=(kc == KC - 1)),
                             reads=[tmp_k[t2], h1_k], writes=[ps_k[3 + hf]])

        norm_phase(0, x_in)
        if dbg:
            dbg_h = dout("dbg_h%d" % l, [128, KC, T], BF16)
            dma(misc_c, dbg_h, Rh[:], reads=[Rh_k])

        for i in range(CCH):
            ba = w_next()
            mm_unit(ba, KC, Rh, Rh_k, (0, 1))
            bb = w_next()
            mm_unit(bb, KC, Rh, Rh_k, (2, 3))
            for hf in range(2):
                ti = tmp_get()
                A(lambda: nc.scalar.activation(out=tmp[ti][:], in_=ps[2 + hf][:], func=AF.Sigmoid), [ps_k[2 + hf]], [tmp_k[ti]])
                V(lambda: nc.vector.tensor_tensor(out=uh[:, i, HALO + hf * 512:HALO + (hf + 1) * 512], in0=ps[hf][:], in1=tmp[ti][:], op=ALU.mult),
                  [ps_k[hf], tmp_k[ti], rb_dead], [uh_k])
        x1s = xsrc[(l, "x1")]
        xs_k = {nme: Tk("xs_" + nme) for nme in XSPEC}
        xd_k = {nme: Tk("xd_" + nme) for nme in XSPEC}
        ut_view = x1s[2048:2560, :].rearrange("r b -> (r b)").rearrange("(p c t) -> p c t", c=CCH, p=128)
        dma(misc_c, ut_view, uh[:, :, UW - 32:UW], reads=[uh_k], writes=[xs_k["x1"]])
        if dbg:
            dbg_u = dout("dbg_u%d" % l, [128, CCH, UW], BF16)
            dma(misc_c, dbg_u, uh[:], reads=[uh_k])

        qt_tiles = [Rb[:, CU_OFF + i * T:CU_OFF + (i + 1) * T] for i in range(4)]
        qt_k = [Tk("qt%d" % i) for i in range(4)]
        qt_c = [m.chan("qt%d_%d" % (i, l)) for i in range(4)]
        vtm = Rb[:, O_OFF:O_OFF + T].rearrange("p (c d) -> p c d", c=8)
        vtm_k = Tk("vtm")
        vtm_c = m.chan("vtm%d" % l)
        qn = 0
        TB = 7
        ptb = ps[TB][:].bitcast(BF16)

        def pviews(dst_tile, srcs, hf, d):
            if d == 1:
                return dst_tile[:, HS(hf)], list(srcs)
            w = 512 // d
            dv = dst_tile.rearrange("p (r i) -> p r i", r=d)[:, :, hf * w:(hf + 1) * w]
            return dv, [s.rearrange("p (i r) -> p r i", r=d) for s in srcs]

        for h in range(NH):
            g, hh = h // 8, h % 8
            d = DIL[g]
            for which in range(3):
                b = w_next()
                banks = (0, 1) if (3 * h + which) % 2 == 0 else (2, 3)
                mm_unit(b, KC, Rh, Rh_k, banks)
                qi = qn % 4
                qn += 1
                dst = qt_tiles[qi]
                if which < 2:
                    for hf, bank in enumerate(banks):
                        tq = tmp_get()
                        A(lambda: nc.scalar.activation(out=tmp[tq][:], in_=ps[bank][:], func=AF.Square), [ps_k[bank]], [tmp_k[tq]])
                        sb = 4 + hf
                        m.op(PE, lambda: nc.tensor.matmul(ps[sb][:], lhsT=onesf[:], rhs=tmp[tq][:], start=True, stop=True),
                             reads=[tmp_k[tq], misc_k], writes=[ps_k[sb]])
                        ti = tmp_get()
                        A(lambda: nc.scalar.activation(out=tmp[ti][:], in_=ps[sb][:], func=AF.Sqrt, bias=128.0 * 1e-6, scale=1.0),
                          [ps_k[sb]], [tmp_k[ti]])
                        V(lambda: nc.vector.reciprocal(out=tmp[ti][:], in_=tmp[ti][:]), [tmp_k[ti]], [tmp_k[ti]])
                        gcol = 0 if which == 0 else 2
                        dv, (pin, rin) = pviews(dst, (ps[bank][:], tmp[ti][:]), hf, d)
                        V(lambda: nc.vector.scalar_tensor_tensor(out=dv, in0=pin, scalar=qkgt[:, gcol:gcol + 1], in1=rin, op0=ALU.mult, op1=ALU.mult),
                          [ps_k[bank], tmp_k[ti], misc_k, rb_dead], [qt_k[qi]])
                    if which == 0:
                        dma(qt_c[qi], qs[h], dst, reads=[qt_k[qi]])
                    else:
                        dma(qt_c[qi], ks[h], dst, reads=[qt_k[qi]])
                        if g == 0:
                            dma(qt_c[qi], x1s[hh * 128:(hh + 1) * 128, :], dst[:, T - 128:T], reads=[qt_k[qi]], writes=[xs_k["x1"]])
                        elif g == 1:
                            k2s = xsrc[(l, "k2")]
                            dma(qt_c[qi], k2s[hh * 128:(hh + 1) * 128, :].rearrange("p (r i) -> p r i", r=4),
                                dst.rearrange("p (r i) -> p r i", r=4)[:, :, 128:256], reads=[qt_k[qi]], writes=[xs_k["k2"]])
                        else:
                            nme = "k3a" if hh < 4 else "k3b"
                            dma(qt_c[qi], xsrc[(l, nme)][(hh % 4) * 128:(hh % 4 + 1) * 128, :], dst, reads=[qt_k[qi]], writes=[xs_k[nme]])
                else:
                    for hf, bank in enumerate(banks):
                        dv, (pin,) = pviews(dst, (ps[bank][:],), hf, d)
                        A(lambda: nc.scalar.copy(out=dv, in_=pin), [ps_k[bank], rb_dead], [qt_k[qi]])
                    for tt in range(8):
                        m.op(PE, lambda: nc.tensor.transpose(ptb[:, tt * 128:(tt + 1) * 128], dst[:, tt * 128:(tt + 1) * 128], identb[:]),
                             reads=[qt_k[qi], misc_k], writes=[ps_k[TB]], signal=(tt == 7))
                    V(lambda: nc.vector.tensor_copy(out=vtm[:].rearrange("p c d -> p (c d)"), in_=ptb), [ps_k[TB], rb_dead], [vtm_k])
                    dma(vtm_c, vs[:, h * 128:(h + 1) * 128].rearrange("(c p) d -> p c d", p=128), vtm[:], reads=[vtm_k])
                    if g == 0:
                        v1 = x1s[1024:2048, :].rearrange("(t e) d -> t e d", e=8)[:, hh, :]
                        dma(vtm_c, v1, vtm[:, 7, :], reads=[vtm_k], writes=[xs_k["x1"]])
                    elif g == 1:
                        v2s = xsrc[(l, "v2")]
                        dma(vtm_c, v2s[:, hh * 128:(hh + 1) * 128].rearrange("(r p) d -> p r d", p=128),
                            vtm[:].rearrange("p (r two) d -> p r two d", two=2)[:, :, 1, :], reads=[vtm_k], writes=[xs_k["v2"]])
                    else:
                        for a, nme in enumerate(("v3a", "v3b")):
                            dma(vtm_c, xsrc[(l, nme)][:, hh * 128:(hh + 1) * 128].rearrange("(c p) d -> p c d", p=128),
                                vtm[:, 4 * a:4 * a + 4, :], reads=[vtm_k], writes=[xs_k[nme]])
        rb_all += qt_k + [vtm_k]
        qkv_done = Tk("qkv_done")
        for c_ in qt_c + [vtm_c]:
            qkv_done.w[c_] = c_.cnt
        if dbg:
            for nme_, t_ in (("dbg_q%d" % l, qs), ("dbg_k%d" % l, ks)):
                o_ = dout(nme_, [NH, 128, T], BF16)
                dma(misc_c, o_, t_, reads=[qkv_done])
            o_ = dout("dbg_v%d" % l, [T, NH * 128], BF16)
            dma(misc_c, o_, vs, reads=[qkv_done])
        if stop == "qkv" and l == layers[-1]:
            break

        for nme, (R, C) in XSPEC.items():
            dstt = xdst[(l, nme)]
            per = 2 * R * C // 128
            zv = dstt[0:2 * R, :].rearrange("r c -> (r c)").rearrange("(p f) -> p f", p=128)
            for o0 in range(0, per, 512):
                w_ = min(512, per - o0)
                dma(misc_c, zv[:, o0:o0 + w_], zrow[:, 0:w_], reads=[misc_k], writes=[])
            xd_k[nme].w[misc_c] = misc_c.cnt
        for nme, (R, C) in XSPEC.items():
            collective(xsrc[(l, nme)].opt(), xdst[(l, nme)][2 * R:10 * R, :].opt(), [xs_k[nme], qkv_done], [xd_k[nme]])
        if l == 0 and 1 in layers:
            gather_upto(-(-units_through("out1" if stop != "qkv" else "qkv1") // 8))
        else:
            gather_upto(ncol)

        hl, hl_k = {}, {}
        for nme, (R, C) in XSPEC.items():
            for back in ((1, 2) if nme in ("k3a", "k3b", "v3a", "v3b") else (1,)):
                key = (R, back)
                if key not in dynbase:
                    dynbase[key] = nc.sync.snap((core + (2 - back)) * R, min_val=0, max_val=9 * R)
                hl[(nme, back)] = dscr("hl_%s_%d_%d" % (nme, back, l), [R, C], BF16)
                hl_k[(nme, back)] = Tk("hl")
                ch_ = m.chan("hl_%s_%d_%d" % (nme, back, l))
                dma(ch_, hl[(nme, back)], xdst[(l, nme)][bass.ds(dynbase[key], R), :], reads=[xd_k[nme]], writes=[hl_k[(nme, back)]])

        def prow(nme, back, row0, nrows):
            return hl[(nme, back)][row0:row0 + nrows, :]

        if stop == "xch":
            break
        if stop == "xch0":
            pass

        hsrc = prow("x1", 1, 2048, 512).rearrange("r b -> (r b)").rearrange("(p c t) -> p c t", c=CCH, p=128)
        dma(misc_c, uh[:, :, 0:HALO], hsrc, reads=[hl_k[("x1", 1)], uh_k], writes=[uh_k])
        V(lambda: nc.vector.tensor_scalar(out=uh[:, :, 0:HALO], in0=uh[:, :, 0:HALO], scalar1=cstt[:, 6, 0:1], scalar2=None, op0=ALU.mult),
          [uh_k, misc_k], [uh_k])
        for i in range(CCH):
            ai = xc_get()
            acc = xc[ai]
            V(lambda: nc.vector.tensor_scalar(out=acc[:], in0=uh[:, i, 2:2 + T], scalar1=cwt[:, i, 0:1], scalar2=cpt[:, 0, i:i + 1],
                                              op0=ALU.mult, op1=ALU.add), [uh_k, misc_k], [xc_k[ai]])
            for j in range(1, CW):
                V(lambda: nc.vector.scalar_tensor_tensor(out=acc[:], in0=uh[:, i, 2 + j:2 + j + T], scalar=cwt[:, i, j:j + 1], in1=acc[:],
                                                         op0=ALU.mult, op1=ALU.add), [uh_k, xc_k[ai], misc_k], [xc_k[ai]])
            for hf in range(2):
                m.op(PE, lambda: nc.tensor.matmul(ps[hf][:], lhsT=onesf[:], rhs=acc[:, HS(hf)], start=(i == 0), stop=(i == CCH - 1)),
                     reads=[xc_k[ai], misc_k], writes=[ps_k[hf]])
                ti = tmp_get()
                A(lambda: nc.scalar.activation(out=tmp[ti][:], in_=acc[:, HS(hf)], func=AF.Square), [xc_k[ai]], [tmp_k[ti]])
                m.op(PE, lambda: nc.tensor.matmul(ps[2 + hf][:], lhsT=onesf[:], rhs=tmp[ti][:], start=(i == 0), stop=(i == CCH - 1)),
                     reads=[tmp_k[ti], misc_k], writes=[ps_k[2 + hf]])
            A(lambda: nc.scalar.copy(out=cuT[:, i, :], in_=acc[:]), [xc_k[ai], qkv_done], [cu_k])
        mi = xc_get()
        ri2 = xc_get(avoid=(mi,))
        mu, rs = xc[mi], xc[ri2]
        for hf in range(2):
            V(lambda: nc.vector.tensor_scalar(out=mu[:, HS(hf)], in0=ps[hf][:], scalar1=1.0 / 2048, scalar2=None, op0=ALU.mult),
              [ps_k[hf]], [xc_k[mi]])
            ti = tmp_get()
            V(lambda: nc.vector.tensor_tensor(out=tmp[ti][:], in0=mu[:, HS(hf)], in1=mu[:, HS(hf)], op=ALU.mult), [xc_k[mi]], [tmp_k[ti]])
            V(lambda: nc.vector.scalar_tensor_tensor(out=rs[:, HS(hf)], in0=ps[2 + hf][:], scalar=1.0 / 2048, in1=tmp[ti][:],
                                                     op0=ALU.mult, op1=ALU.subtract), [ps_k[2 + hf], tmp_k[ti]], [xc_k[ri2]])
            A(lambda: nc.scalar.activation(out=rs[:, HS(hf)], in_=rs[:, HS(hf)], func=AF.Sqrt, bias=1e-5, scale=1.0), [xc_k[ri2]], [xc_k[ri2]])
            V(lambda: nc.vector.reciprocal(out=rs[:, HS(hf)], in_=rs[:, HS(hf)]), [xc_k[ri2]], [xc_k[ri2]])
        for i in range(CCH):
            for hf in range(2):
                ti = tmp_get()
                V(lambda: nc.vector.tensor_tensor(out=tmp[ti][:], in0=cuT[:, i, HS(hf)], in1=mu[:, HS(hf)], op=ALU.subtract),
                  [cu_k, xc_k[mi]], [tmp_k[ti]])
                V(lambda: nc.vector.tensor_tensor(out=tmp[ti][:], in0=tmp[ti][:], in1=rs[:, HS(hf)], op=ALU.mult), [tmp_k[ti], xc_k[ri2]], [tmp_k[ti]])
                A(lambda: nc.scalar.activation(out=cuT[:, i, HS(hf)], in_=tmp[ti][:], func=AF.Silu, bias=cpt[:, 2, i:i + 1], scale=cpt[:, 1, i:i + 1]),
                  [tmp_k[ti], misc_k], [cu_k])
        if dbg:
            dbg_cu = dout("dbg_cu%d" % l, [128, CCH, T], BF16)
            dma(misc_c, dbg_cu, cuT, reads=[cu_k])

        if stop == "conv":
            break
        att_k = Tk("att")
        u_dead = retire(uh_k)

        def load_exp(src_ap, ncols_):
            bi = xc_get()
            dma(xc_c[bi], xc[bi][:, 0:ncols_], src_ap, writes=[xc_k[bi]])
            A(lambda: nc.scalar.activation(out=xc[bi][:, 0:ncols_], in_=xc[bi][:, 0:ncols_], func=AF.Exp), [xc_k[bi]], [xc_k[bi]])
            return bi

        for h0 in (0, 8):
            bi = load_exp(biasD[:, h0:h0 + 8, :].rearrange("p h q -> p (h q)"), 1024)
            for hh in range(8):
                V(lambda: nc.vector.tensor_tensor(out=MDP[:, h0 + hh, 0:128], in0=xc[bi][:, hh * 128:(hh + 1) * 128], in1=cstt[:, 0, :], op=ALU.mult),
                  [xc_k[bi], misc_k, att_k, u_dead], [att_k])
            bi = load_exp(biasP[:, h0:h0 + 8, :].rearrange("p h q -> p (h q)"), 1024)
            for hh in range(8):
                V(lambda: nc.vector.tensor_tensor(out=MDP[:, h0 + hh, 128:256], in0=xc[bi][:, hh * 128:(hh + 1) * 128], in1=cstt[:, 1, :], op=ALU.mult),
                  [xc_k[bi], misc_k, att_k, u_dead], [att_k])
        for w3 in range(3):
            bi = load_exp(biasG[:, :, w3, :], 1024) if False else None
            bi = xc_get()
            dma(xc_c[bi], xc[bi][:].rearrange("p (h q) -> p h q", h=8), biasG[:, :, w3, :], writes=[xc_k[bi]])
            A(lambda: nc.scalar.activation(out=xc[bi][:], in_=xc[bi][:], func=AF.Exp), [xc_k[bi]], [xc_k[bi]])
            for hh in range(8):
                V(lambda: nc.vector.tensor_tensor(out=MG3[:, hh, w3, :], in0=xc[bi][:, hh * 128:(hh + 1) * 128], in1=cstt[:, 3 + w3, :], op=ALU.mult),
                  [xc_k[bi], misc_k, att_k, u_dead], [att_k])

        hd_n = ("Q", "K0", "KA", "V0", "VA")
        hd_k = {n_: Tk(n_) for n_ in hd_n}
        hd_c = {n_: m.chan("h%s%d" % (n_, l)) for n_ in hd_n}
        PT_k = [Tk("PT%d" % i) for i in range(NPT)]
        rb_all += [att_k] + list(hd_k.values()) + PT_k
        NB, DBK = (0, 1), (2, 3)
        SBK = (4, 5, 6)
        rr = {"sb": 0, "pt": 0}
        nacc_i = xc_get()
        dacc_i = xc_get(avoid=(nacc_i,))
        Nacc, Dacc = xc[nacc_i], xc[dacc_i]

        def att_tile(Kap, Ktk, nk, Vap, Vtk, q0, nq, mask_ap, first, vcol=None):
            sb = SBK[rr["sb"] % 3]
            rr["sb"] += 1
            m.op(PE, lambda: nc.tensor.matmul(ps[sb][0:nk, 0:nq], lhsT=Kap, rhs=Qh[:, q0:q0 + nq], start=True, stop=True),
                 reads=[Ktk, hd_k["Q"]], writes=[ps_k[sb]])
            pi = rr["pt"] % NPT
            rr["pt"] += 1
            ti = tmp_get()
            A(lambda: nc.scalar.activation(out=tmp[ti][0:nk, 0:nq], in_=ps[sb][0:nk, 0:nq], func=AF.Exp), [ps_k[sb]], [tmp_k[ti]])
            if vcol is None:
                V(lambda: nc.vector.tensor_tensor(out=PT[pi][0:nk, 0:nq], in0=tmp[ti][0:nk, 0:nq], in1=mask_ap, op=ALU.mult),
                  [tmp_k[ti], att_k, u_dead], [PT_k[pi]])
            else:
                V(lambda: nc.vector.scalar_tensor_tensor(out=PT[pi][0:nk, 0:nq], in0=tmp[ti][0:nk, 0:nq], scalar=cstt[0:nk, 6, vcol:vcol + 1],
                                                         in1=mask_ap, op0=ALU.mult, op1=ALU.mult), [tmp_k[ti], att_k, u_dead, misc_k], [PT_k[pi]])
            done = 0
            while done < nq:
                cur = q0 + done
                bk = cur // 512
                w_ = min(nq - done, 512 - (cur % 512))
                osl = slice(cur % 512, cur % 512 + w_)
                st = first[bk]
                first[bk] = False
                m.op(PE, lambda: nc.tensor.matmul(ps[NB[bk]][:, osl], lhsT=Vap, rhs=PT[pi][0:nk, done:done + w_], start=st, stop=False,
                                                  skip_group_check=True), reads=[Vtk, PT_k[pi]], writes=[ps_k[NB[bk]]])
                m.op(PE, lambda: nc.tensor.matmul(ps[DBK[bk]][:, osl], lhsT=onesb[0:nk, :], rhs=PT[pi][0:nk, done:done + w_], start=st, stop=False,
                                                  skip_group_check=True), reads=[misc_k, PT_k[pi]], writes=[ps_k[DBK[bk]]])
                done += w_

        V0t = V0[:, 0:T].rearrange("p (c d) -> p c d", c=8)
        for hh in range(8):
            for g in range(3):
                h = g * 8 + hh
                d = DIL[g]
                dma(hd_c["Q"], Qh, qs[h], reads=[qkv_done, u_dead], writes=[hd_k["Q"]])
                dma(hd_c["K0"], K0, ks[h], reads=[qkv_done, u_dead], writes=[hd_k["K0"]])
                vcol = vs[:, h * 128:(h + 1) * 128]
                dma(hd_c["V0"], V0t, vcol.rearrange("(c p) d -> p c d", p=128), reads=[qkv_done, u_dead], writes=[hd_k["V0"]])
                if g == 0:
                    dma(hd_c["KA"], KA[:, 0:128], prow("x1", 1, hh * 128, 128), reads=[hl_k[("x1", 1)], u_dead], writes=[hd_k["KA"]])
                    dma(hd_c["VA"], VA[:, 0:128], prow("x1", 1, 1024, 1024).rearrange("(t e) d -> t e d", e=8)[:, hh, :],
                        reads=[hl_k[("x1", 1)], u_dead], writes=[hd_k["VA"]])
                elif g == 1:
                    dma(hd_c["KA"], KA[:, 0:512], prow("k2", 1, hh * 128, 128), reads=[hl_k[("k2", 1)], u_dead], writes=[hd_k["KA"]])
                    dma(hd_c["VA"], VA[:, 0:512].rearrange("p (r d) -> p r d", r=4),
                        prow("v2", 1, 0, 512)[:, hh * 128:(hh + 1) * 128].rearrange("(r p) d -> p r d", p=128),
                        reads=[hl_k[("v2", 1)], u_dead], writes=[hd_k["VA"]])
                else:
                    nme = "k3a" if hh < 4 else "k3b"
                    for back in (1, 2):
                        dma(hd_c["KA"], KA[:, (back - 1) * T:back * T], prow(nme, back, (hh % 4) * 128, 128),
                            reads=[hl_k[(nme, back)], u_dead], writes=[hd_k["KA"]])
                        for a, vn in enumerate(("v3a", "v3b")):
                            dma(hd_c["VA"], VA[:, (back - 1) * T + a * 512:(back - 1) * T + (a + 1) * 512].rearrange("p (c d) -> p c d", c=4),
                                prow(vn, back, 0, 512)[:, hh * 128:(hh + 1) * 128].rearrange("(c p) d -> p c d", p=128),
                                reads=[hl_k[(vn, back)], u_dead], writes=[hd_k["VA"]])
                first = [True, True]
                if g == 0:
                    att_tile(KA[:, 0:128], hd_k["KA"], 128, VA[:, 0:128], hd_k["VA"], 0, 128, MDP[:, h, 128:256], first, vcol=0)
                    for j in range(8):
                        nq = 256 if j < 7 else 128
                        att_tile(K0[:, j * 128:(j + 1) * 128], hd_k["K0"], 128, V0t[:, j, :], hd_k["V0"], j * 128, nq, MDP[:, h, 0:nq], first)
                elif g == 1:
                    for r in range(4):
                        att_tile(KA[:, r * 128:(r + 1) * 128], hd_k["KA"], 128, VA[:, r * 128:(r + 1) * 128], hd_k["VA"], r * 256, 128,
                                 MDP[:, h, 128:256], first, vcol=0)
                        att_tile(K0[:, r * 256:r * 256 + 128], hd_k["K0"], 128, V0t[:, 2 * r, :], hd_k["V0"], r * 256, 256, MDP[:, h, :], first)
                        att_tile(K0[:, r * 256 + 128:r * 256 + 256], hd_k["K0"], 128, V0t[:, 2 * r + 1, :], hd_k["V0"], r * 256 + 128, 128,
                                 MDP[:, h, 0:128], first)
                else:
                    for pr_ in range(8):
                        cs = slice(pr_ * 128, (pr_ + 1) * 128)
                        att_tile(KA[:, T + pr_ * 128:T + (pr_ + 1) * 128], hd_k["KA"], 128, VA[:, T + pr_ * 128:T + (pr_ + 1) * 128], hd_k["VA"],
                                 pr_ * 128, 128, MG3[:, hh, 2, :], first, vcol=1)
                        att_tile(KA[:, cs], hd_k["KA"], 128, VA[:, cs], hd_k["VA"], pr_ * 128, 128, MG3[:, hh, 1, :], first, vcol=0)
                        att_tile(K0[:, cs], hd_k["K0"], 128, V0t[:, pr_, :], hd_k["V0"], pr_ * 128, 128, MG3[:, hh, 0, :], first)
                for bk in range(2):
                    if g == 0:
                        A(lambda: nc.scalar.copy(out=Nacc[:, HS(bk)], in_=ps[NB[bk]][:]), [ps_k[NB[bk]]], [xc_k[nacc_i]])
                        A(lambda: nc.scalar.copy(out=Dacc[:, HS(bk)], in_=ps[DBK[bk]][:]), [ps_k[DBK[bk]]], [xc_k[dacc_i]])
                    else:
                        hd2 = d // 2
                        for accT, acci, bank in ((Nacc, nacc_i, NB[bk]), (Dacc, dacc_i, DBK[bk])):
                            av = accT.rearrange("p (i r) -> p r i", r=d)[:, bk * hd2:(bk + 1) * hd2, :]
                            pv = ps[bank][:].rearrange("p (r i) -> p r i", r=hd2)
                            V(lambda: nc.vector.tensor_tensor(out=av, in0=av, in1=pv, op=ALU.add), [ps_k[bank], xc_k[acci]], [xc_k[acci]])
            V(lambda: nc.vector.reciprocal(out=Dacc[:], in_=Dacc[:]), [xc_k[dacc_i]], [xc_k[dacc_i]])
            V(lambda: nc.vector.tensor_tensor(out=oT[:, hh, :], in0=Nacc[:], in1=Dacc[:], op=ALU.mult), [xc_k[nacc_i], xc_k[dacc_i], qkv_done], [o_k])
        if dbg:
            dbg_o = dout("dbg_o%d" % l, [128, 8, T], BF16)
            dma(misc_c, dbg_o, oT, reads=[o_k])

        if stop == "attn":
            break
        mg_k = [Tk("mg0"), Tk("mg1")]
        mg_c = [m.chan("mg0_%d" % l), m.chan("mg1_%d" % l)]
        att_done = retire(att_k, *hd_k.values(), *PT_k)
        rb_all += mg_k
        for j in range(KC):
            bnk = [(0, 1), (2, 3), (4, 5), (6, 7)]
            specs = [(KC, Rh, Rh_k), (KC, Rh, Rh_k), (CCH, cuT, cu_k), (8, oT, o_k)]
            for (nkc, r3, rk), bk in zip(specs, bnk):
                b = w_next()
                mm_unit(b, nkc, r3, rk, bk)
            gi = j % 2
            for hf in range(2):
                t1, t2 = tmp_get(), tmp_get()
                A(lambda: nc.scalar.activation(out=tmp[t1][:], in_=ps[0 + hf][:], func=AF.Sigmoid), [ps_k[0 + hf]], [tmp_k[t1]])
                A(lambda: nc.scalar.activation(out=tmp[t2][:], in_=ps[2 + hf][:], func=AF.Sigmoid), [ps_k[2 + hf]], [tmp_k[t2]])
                V(lambda: nc.vector.tensor_tensor(out=tmp[t1][:], in0=tmp[t1][:], in1=ps[4 + hf][:], op=ALU.mult), [tmp_k[t1], ps_k[4 + hf]], [tmp_k[t1]])
                V(lambda: nc.vector.tensor_tensor(out=tmp[t2][:], in0=tmp[t2][:], in1=ps[6 + hf][:], op=ALU.mult), [tmp_k[t2], ps_k[6 + hf]], [tmp_k[t2]])
                V(lambda: nc.vector.tensor_tensor(out=mgt[gi][:, HS(hf)], in0=tmp[t1][:], in1=tmp[t2][:], op=ALU.add),
                  [tmp_k[t1], tmp_k[t2], att_done], [mg_k[gi]])
            dma(mg_c[gi], mscr[j], mgt[gi], reads=[mg_k[gi]])
        mdone = Tk("mdone")
        for c_ in mg_c:
            mdone.w[c_] = c_.cnt
        if dbg:
            dbg_m = dout("dbg_m%d" % l, [KC, 128, T], BF16)
            dma(misc_c, dbg_m, mscr, reads=[mdone])

        dma(misc_c, Rh[:], mscr.rearrange("k p t -> p k t"), reads=[mdone, Rh_k], writes=[Rh_k])
        rmw_avoid = ()
        rmw_src = x_in
        for j in range(KC):
            b = w_next()
            banks = (0, 1) if j % 2 == 0 else (2, 3)
            mm_unit(b, KC, Rh, Rh_k, banks)
            x_rmw(j, banks, GT1)
        if stop == "mix" and l == layers[-1]:
            break

        def x_cur(j):
            return outT[j * 128:(j + 1) * 128, :]

        rmw_src = x_cur
        mix_dead = retire(*rb_all)
        if l == 1:
            dma(misc_c, selt, sel_in, reads=[mix_dead], writes=[h1_k])
            dma(misc_c, wrt, wr_in, reads=[mix_dead], writes=[h1_k])
        norm_phase(1, x_cur, router=(l == 1))
        if l == 0:
            for (f0, f1) in FFG:
                nf = f1 - f0
                for f in range(nf):
                    s0 = 0 if f % 2 == 0 else 4
                    b = w_next()
                    mm_unit(b, KC, Rh, Rh_k, (s0, s0 + 1))
                    b = w_next()
                    mm_unit(b, KC, Rh, Rh_k, (s0 + 2, s0 + 3))
                    for hf in range(2):
                        ti = tmp_get()
                        A(lambda: nc.scalar.activation(out=tmp[ti][:], in_=ps[s0 + hf][:], func=AF.Silu), [ps_k[s0 + hf]], [tmp_k[ti]])
                        V(lambda: nc.vector.tensor_tensor(out=h1[:, f, HS(hf)], in0=tmp[ti][:], in1=ps[s0 + 2 + hf][:], op=ALU.mult),
                          [tmp_k[ti], ps_k[s0 + 2 + hf], mix_dead], [h1_k])
                for j in range(KC):
                    b = w_next()
                    banks = (0, 1) if j % 2 == 0 else (2, 3)
                    mm_unit(b, nf, h1, h1_k, banks)
                    x_rmw(j, banks, GT2)
        else:
            for hf in range(2):
                A(lambda: nc.scalar.copy(out=LT[:, HS(hf)], in_=ps[3 + hf][0:8, :]), [ps_k[3 + hf]], [h1_k])
            for blk in range(8):
                m.op(PE, lambda: nc.tensor.transpose(ps[5][:, blk * 8:(blk + 1) * 8], LT[:, blk * 128:(blk + 1) * 128], cstt[0:8, 2, 0:8]),
                     reads=[h1_k, misc_k], writes=[ps_k[5]], signal=(blk == 7))
            V(lambda: nc.vector.tensor_copy(out=Ltm[:], in_=ps[5][:, 0:64]), [ps_k[5]], [h1_k])
            for blk in range(8):
                Lb = Ltm[:, blk * 8:(blk + 1) * 8]
                Cb = ctm[:, blk * 8:(blk + 1) * 8]
                s = small
                V(lambda: nc.vector.tensor_reduce(out=s[:, 0:1], in_=Lb, axis=AX.X, op=ALU.max), [h1_k], [sm_k])
                V(lambda: nc.vector.tensor_scalar(out=Cb, in0=Lb, scalar1=s[:, 0:1], scalar2=-1e30, op0=ALU.is_equal, op1=ALU.mult), [h1_k, sm_k], [h1_k])
                V(lambda: nc.vector.tensor_tensor(out=Cb, in0=Cb, in1=Lb, op=ALU.add), [h1_k], [h1_k])
                V(lambda: nc.vector.tensor_reduce(out=s[:, 1:2], in_=Cb, axis=AX.X, op=ALU.max), [h1_k], [sm_k])
                V(lambda: nc.vector.tensor_scalar(out=s[:, 2:3], in0=s[:, 0:1], scalar1=-1.0, scalar2=None, op0=ALU.mult), [sm_k], [sm_k])
                V(lambda: nc.vector.tensor_scalar(out=Cb, in0=Lb, scalar1=s[:, 1:2], scalar2=None, op0=ALU.is_ge), [h1_k, sm_k], [h1_k])
                A(lambda: nc.scalar.activation(out=s[:, 8:16], in_=Lb, func=AF.Exp, bias=s[:, 2:3], scale=1.0), [h1_k, sm_k], [sm_k])
                V(lambda: nc.vector.tensor_tensor(out=Cb, in0=Cb, in1=s[:, 8:16], op=ALU.mult), [h1_k, sm_k], [h1_k])
                V(lambda: nc.vector.tensor_reduce(out=s[:, 3:4], in_=Cb, axis=AX.X, op=ALU.add), [h1_k], [sm_k])
                V(lambda: nc.vector.reciprocal(out=s[:, 3:4], in_=s[:, 3:4]), [sm_k], [sm_k])
                V(lambda: nc.vector.tensor_scalar(out=Cb, in0=Cb, scalar1=s[:, 3:4], scalar2=None, op0=ALU.mult), [h1_k, sm_k], [h1_k])
            for blk in range(8):
                bank = 3 + blk // 4
                m.op(PE, lambda: nc.tensor.transpose(ps[bank][0:8, (blk % 4) * 128:(blk % 4 + 1) * 128], ctm[:, blk * 8:(blk + 1) * 8], cstt[:, 2, :]),
                     reads=[h1_k, misc_k], writes=[ps_k[bank]], signal=(blk % 4 == 3))
            for hf in range(2):
                A(lambda: nc.scalar.copy(out=combT[:, HS(hf)], in_=ps[3 + hf][0:8, :]), [ps_k[3 + hf]], [h1_k])
            if dbg:
                dbg_c = dout("dbg_comb", [8, T])
                dma(misc_c, dbg_c, combT, reads=[h1_k])
            for e in range(EXP):
                ci = xc_get()
                crep = xc[ci]
                for hf in range(2):
                    m.op(PE, lambda: nc.tensor.matmul(ps[6 + hf][:], lhsT=selt[:, e * 128:(e + 1) * 128], rhs=combT[:, HS(hf)], start=True, stop=True),
                         reads=[h1_k], writes=[ps_k[6 + hf]])
                    A(lambda: nc.scalar.copy(out=crep[:, HS(hf)], in_=ps[6 + hf][:]), [ps_k[6 + hf]], [xc_k[ci]])
                rmw_avoid = (ci,)
                for f in range(DFE_C):
                    s0 = 0 if f % 2 == 0 else 4
                    b = w_next()
                    mm_unit(b, KC, Rh, Rh_k, (s0, s0 + 1))
                    b = w_next()
                    mm_unit(b, KC, Rh, Rh_k, (s0 + 2, s0 + 3))
                    for hf in range(2):
                        ti = tmp_get()
                        A(lambda: nc.scalar.activation(out=tmp[ti][:], in_=ps[s0 + hf][:], func=AF.Silu), [ps_k[s0 + hf]], [tmp_k[ti]])
                        V(lambda: nc.vector.tensor_tensor(out=tmp[ti][:], in0=tmp[ti][:], in1=ps[s0 + 2 + hf][:], op=ALU.mult),
                          [tmp_k[ti], ps_k[s0 + 2 + hf]], [tmp_k[ti]])
                        V(lambda: nc.vector.tensor_tensor(out=h1[:, f, HS(hf)], in0=tmp[ti][:], in1=crep[:, HS(hf)], op=ALU.mult),
                          [tmp_k[ti], xc_k[ci], mix_dead], [h1_k])
                for j in range(KC):
                    b = w_next()
                    banks = (0, 1) if j % 2 == 0 else (2, 3)
                    mm_unit(b, DFE_C, h1, h1_k, banks)
                    x_rmw(j, banks, GT2)
            rmw_avoid = ()

    gather_upto(ncol)
    for c_ in m.chans:
        if c_.cnt:
            m._wait(m.sp, c_, c_.cnt)
    for e_ in (m.pe, m.act, m.dve):
        if e_.cnt:
            m._wait(m.sp, e_, e_.cnt)
    m.close()
    return m


def host_inputs(inp, cfg):
    shards, ncol = build_units(inp, cfg)
    bd, bp = bias_layout(inp["rel_bias"])
    k = np.arange(128)[:, None]
    q = np.arange(128)[None, :]
    same = (k // 64) == (q // 64)
    dl = [(q % 64) - (k % 64), (q % 64) - (k % 64) + 64, (q % 64) + 128 - (k % 64)]
    m3 = [same & (dl[0] >= 0), same, same & (dl[2] <= 128)]
    bg = np.zeros((128, 8, 3, 128), np.float32)
    for w3 in range(3):
        idx3 = t5_bucket_np(np.clip(dl[w3], 0, 128) * 16)
        for hh in range(8):
            bg[:, hh, w3, :] = inp["rel_bias"][idx3, 16 + hh]
    maskD = (q >= k).astype(np.float32)
    maskP = (k >= q).astype(np.float32)
    ident = np.eye(128, dtype=np.float32)
    sel = np.zeros((8, EXP * 128), np.float32)
    for e in range(EXP):
        sel[e, e * 128:(e + 1) * 128] = 1.0
    wr = np.ascontiguousarray(inp["moe_router"][0].reshape(KC, 128, EXP).transpose(1, 0, 2))
    common = {
        "bada": np.stack([vec_pc(inp["b_ada"][l]) for l in range(2)]),
        "gmix": np.stack([vec_pc(inp["g_mix"][l]) for l in range(2)]),
        "gffn": np.stack([vec_pc(inp["g_ffn"][l]) for l in range(2)]),
        "convw": np.stack([np.ascontiguousarray(inp["conv_w"][l].T.reshape(CCH, 128, CW).transpose(1, 0, 2)) for l in range(2)]),
        "convp": np.stack([np.stack([vec_pc(inp["conv_b"][l]), vec_pc(inp["conv_ln_g"][l]), vec_pc(inp["conv_ln_b"][l])], axis=1) for l in range(2)]),
        "qkg": np.stack([np.stack([inp["q_gain"][l], inp["k_gain"][l]], axis=1) for l in range(2)]),
        "biasD": bd, "biasP": np.ascontiguousarray(bp[:, 0:16, :]), "biasG": bg, "sel": sel, "wr": wr,
    }
    in_maps = []
    for c in range(NCORE):
        b, r = c // 4, c % 4
        v1, v2 = float(r >= 1), float(r >= 2)
        cstc = np.zeros((128, 7, 128), np.float32)
        cstc[:, 0] = maskD
        cstc[:, 1] = maskP
        cstc[:, 2] = ident
        for w3 in range(3):
            cstc[:, 3 + w3] = m3[w3].astype(np.float32)
        cstc[:, 6, 0] = v1
        cstc[:, 6, 1] = v2
        d_ = dict(common)
        d_["xT"] = np.ascontiguousarray(inp["x"][b, r * T:(r + 1) * T, :].T)
        d_["wsh"] = shards[c]
        d_["cvec"] = vec_pc(inp["c"][b])
        d_["cst"] = cstc
        in_maps.append({k_: np.ascontiguousarray(v_, dtype=np.float32) for k_, v_ in d_.items()})
    return in_maps, ncol


def run(inp, cfg, trace=False):
    in_maps, ncol = host_inputs(inp, cfg)
    m = build(cfg, ncol)
    res = run_bass_kernel_spmd(m.nc, in_maps, core_ids=list(range(NCORE)), trace=trace)
    return res, m


def kernel(**inputs):
    inp = {k: np.asarray(v) for k, v in inputs.items()}
    cfg = {"layers": [0, 1], "stop": "all"}
    res, _ = run(inp, cfg)
    out = np.empty((2, 4 * T, D), np.float32)
    for c in range(NCORE):
        b, r = c // 4, c % 4
        out[b, r * T:(r + 1) * T, :] = res.results[c]["outT"].T
    return out
```
